# Optimizing a Trainium2 kernel written in Bass

```python
import math
import jax, jax.numpy as jnp
from jax import lax
import numpy as np

D_MODEL = 1024
BATCH = 4
SEQ = 8192
DEPTH = 4

SB_HEADS = 8
SB_HEAD_DIM = 64
SB_WIDTH = SB_HEADS * SB_HEAD_DIM
Q_BLOCK = 128
DN_HEADS = 4
DN_HEAD_DIM = 128
DN_WIDTH = DN_HEADS * DN_HEAD_DIM
CONV_K = 4
CHUNK = 64
N_EXPERTS = 16
N_GROUPS = 4
EXPERTS_PER_GROUP = N_EXPERTS // N_GROUPS
TOP_K = 2
D_EXPERT = 512
ROUTE_BLOCK = 128
DEEPNORM_ALPHA = (2 * DEPTH) ** 0.25
DEEPNORM_BETA = (8 * DEPTH) ** -0.25
LN_EPS = 1e-5
NORM_EPS = 1e-6

PROJ_SPLITS = (SB_WIDTH, SB_WIDTH, SB_WIDTH, DN_WIDTH, DN_WIDTH, DN_WIDTH, DN_WIDTH, DN_HEADS, DN_HEADS, D_MODEL, D_MODEL)
VALUE_COLS = (2, 5)
PROJ_WIDTH = sum(PROJ_SPLITS)

kernel_name = "stickbreak_gdn_gated_hybrid_moe"


def _split(t, sizes):
    offs = np.cumsum(sizes)[:-1].tolist()
    return jnp.split(t, offs, axis=-1)


def layer_norm(x, g, b):
    xf = x.astype(jnp.float32)
    mu = jnp.mean(xf, axis=-1, keepdims=True)
    xc = xf - mu
    var = jnp.mean(xc * xc, axis=-1, keepdims=True)
    return (xc * lax.rsqrt(var + LN_EPS) * g.astype(jnp.float32) + b.astype(jnp.float32)).astype(x.dtype)


def l2_normalize(t):
    return t * lax.rsqrt(jnp.sum(t * t, axis=-1, keepdims=True) + NORM_EPS)


def causal_depthwise_conv(u, w):
    c = u.shape[-1]
    return lax.conv_general_dilated(u, w[:, None, :].astype(u.dtype), window_strides=(1,), padding=[(w.shape[0] - 1, 0)], dimension_numbers=("NWC", "WIO", "NWC"), feature_group_count=c)


def stick_breaking_attention(q, k, v):
    s_len, d = q.shape[2], q.shape[3]
    scale = d ** -0.5
    qf, kf = q.astype(jnp.float32), k.astype(jnp.float32)
    outs = []
    for i in range(s_len // Q_BLOCK):
        start, end = i * Q_BLOCK, (i + 1) * Q_BLOCK
        z = jnp.einsum("bhtd,bhsd->bhts", qf[:, :, start:end], kf[:, :, :end]) * scale
        t_idx = start + jnp.arange(Q_BLOCK)[:, None]
        s_idx = jnp.arange(end)[None, :]
        mask = s_idx < t_idx
        log_beta = jax.nn.log_sigmoid(z)
        log_keep = jnp.where(mask, jax.nn.log_sigmoid(-z), 0.0)
        rest = lax.cumsum(log_keep, axis=3, reverse=True) - log_keep
        a = jnp.where(mask, jnp.exp(log_beta + rest), 0.0)
        outs.append(jnp.einsum("bhts,bhsd->bhtd", a.astype(v.dtype), v[:, :, :end]))
    return jnp.concatenate(outs, axis=2)


def gated_delta_rule(q, k, v, beta, g):
    bn, s_len, h, dk = q.shape
    dv = v.shape[-1]
    n = s_len // CHUNK
    chunks = lambda t: t.reshape(bn, n, CHUNK, h, -1).transpose(0, 3, 1, 2, 4)
    q, k, v = chunks(q), chunks(k), chunks(v)
    beta = beta.reshape(bn, n, CHUNK, h).transpose(0, 3, 1, 2)
    gc = jnp.cumsum(g.reshape(bn, n, CHUNK, h).transpose(0, 3, 1, 2), axis=-1)
    idx = jnp.arange(CHUNK)
    incl = idx[:, None] >= idx[None, :]
    strict = idx[:, None] > idx[None, :]
    gamma = jnp.exp(jnp.where(incl, gc[..., :, None] - gc[..., None, :], -jnp.inf))
    kk = jnp.einsum("bhnid,bhnjd->bhnij", k, k)
    a_low = jnp.where(strict, beta[..., :, None] * kk * gamma, 0.0)
    t_mat = a_low + jnp.eye(CHUNK, dtype=a_low.dtype)
    solve = lambda rhs: lax.linalg.triangular_solve(t_mat, rhs, left_side=True, lower=True, unit_diagonal=True)
    u = solve(beta[..., None] * v)
    w = solve(beta[..., None] * k * jnp.exp(gc)[..., None])
    qk = jnp.einsum("bhnid,bhnjd->bhnij", q, k) * gamma
    q_dec = q * jnp.exp(gc)[..., None]
    k_dec = k * jnp.exp(gc[..., -1:] - gc)[..., None]
    last = jnp.exp(gc[..., -1])

    def step(state, inp):
        u_c, w_c, qk_c, qd_c, kd_c, last_c = inp
        v_new = u_c - jnp.einsum("bhcd,bhde->bhce", w_c, state)
        o_c = jnp.einsum("bhcd,bhde->bhce", qd_c, state) + jnp.einsum("bhij,bhje->bhie", qk_c, v_new)
        state = last_c[..., None, None] * state + jnp.einsum("bhcd,bhce->bhde", kd_c, v_new)
        return state, o_c

    mv = lambda t: jnp.moveaxis(t, 2, 0)
    s0 = jnp.zeros((bn, h, dk, dv), jnp.float32)
    _, o = lax.scan(step, s0, (mv(u), mv(w), mv(qk), mv(q_dec), mv(k_dec), mv(last)))
    return o.transpose(1, 0, 3, 2, 4).reshape(bn, s_len, h, dv)


def hybrid_mixer(x, w_in, conv_w, a_log, dt_bias, onorm_g, w_br_a, w_br_b, w_o):
    bn, s_len, _ = x.shape
    proj = x @ w_in
    q_a, k_a, v_a, q_b, k_b, v_b, z_b, b_b, a_b, g_a, g_b = _split(proj, PROJ_SPLITS)
    sb = lambda t: t.reshape(bn, s_len, SB_HEADS, SB_HEAD_DIM).transpose(0, 2, 1, 3)
    o_a = stick_breaking_attention(sb(q_a), sb(k_a), sb(v_a))
    o_a = o_a.transpose(0, 2, 1, 3).reshape(bn, s_len, SB_WIDTH)
    qkv = jax.nn.silu(causal_depthwise_conv(jnp.concatenate([q_b, k_b, v_b], axis=-1), conv_w)).astype(jnp.float32)
    q_b, k_b, v_b = _split(qkv, (DN_WIDTH, DN_WIDTH, DN_WIDTH))
    dn = lambda t: t.reshape(bn, s_len, DN_HEADS, DN_HEAD_DIM)
    q_b = l2_normalize(dn(q_b)) * (DN_HEAD_DIM ** -0.5)
    k_b = l2_normalize(dn(k_b))
    beta = jax.nn.sigmoid(b_b.astype(jnp.float32))
    g = -jnp.exp(a_log.astype(jnp.float32)) * jax.nn.softplus(a_b.astype(jnp.float32) + dt_bias.astype(jnp.float32))
    o_b = gated_delta_rule(q_b, k_b, dn(v_b), beta, g)
    o_b = o_b * lax.rsqrt(jnp.mean(o_b * o_b, axis=-1, keepdims=True) + NORM_EPS) * onorm_g.astype(jnp.float32)
    o_b = (o_b * jax.nn.silu(dn(z_b).astype(jnp.float32))).reshape(bn, s_len, DN_WIDTH).astype(x.dtype)
    merged = jax.nn.sigmoid(g_a) * (o_a @ w_br_a) + jax.nn.sigmoid(g_b) * (o_b @ w_br_b)
    return merged @ w_o


def grouped_moe(h, w_router, router_bias, w_gate, w_up, w_down):
    bn, s_len, d = h.shape
    n_tok = bn * s_len
    xf = h.reshape(n_tok, d)
    probs = jax.nn.softmax(xf.astype(jnp.float32) @ w_router.astype(jnp.float32), axis=-1)
    sel = (probs + router_bias.astype(jnp.float32)).reshape(n_tok, N_GROUPS, EXPERTS_PER_GROUP)
    grp_score = jnp.sum(lax.top_k(sel, 2)[0], axis=-1)
    g_idx = jnp.argmax(grp_score, axis=-1)
    in_grp = jnp.take_along_axis(sel, g_idx[:, None, None], axis=1)[:, 0]
    _, local = lax.top_k(in_grp, TOP_K)
    e_idx = g_idx[:, None] * EXPERTS_PER_GROUP + local
    wts = jnp.take_along_axis(probs, e_idx, axis=-1)
    wts = wts / jnp.sum(wts, axis=-1, keepdims=True)
    m = n_tok * TOP_K
    e_flat = e_idx.reshape(m).astype(jnp.int32)
    tok_flat = jnp.repeat(jnp.arange(n_tok, dtype=jnp.int32), TOP_K)
    w_flat = wts.reshape(m)
    order = jnp.argsort(e_flat)
    e_sorted = e_flat[order]
    counts = jnp.zeros((N_EXPERTS,), jnp.int32).at[e_flat].add(1)
    padded = (counts + ROUTE_BLOCK - 1) // ROUTE_BLOCK * ROUTE_BLOCK
    starts = jnp.cumsum(counts) - counts
    pends = jnp.cumsum(padded)
    pstarts = pends - padded
    dest = pstarts[e_sorted] + (jnp.arange(m, dtype=jnp.int32) - starts[e_sorted])
    p_rows = m + N_EXPERTS * ROUTE_BLOCK
    row_tok = jnp.full((p_rows,), n_tok, jnp.int32).at[dest].set(tok_flat[order])
    row_w = jnp.zeros((p_rows,), jnp.float32).at[dest].set(w_flat[order])
    n_blk = p_rows // ROUTE_BLOCK
    blk_exp = jnp.minimum(jnp.searchsorted(pends, jnp.arange(n_blk, dtype=jnp.int32) * ROUTE_BLOCK, side="right"), N_EXPERTS - 1)
    xs = jnp.concatenate([xf, jnp.zeros((1, d), xf.dtype)], axis=0)[row_tok].reshape(n_blk, ROUTE_BLOCK, d)

    def expert_block(args):
        xb, e = args
        hid = jax.nn.silu(xb @ w_gate[e]) * (xb @ w_up[e])
        return hid @ w_down[e]

    ys = lax.map(expert_block, (xs, blk_exp)).reshape(p_rows, d)
    ys = ys * row_w[:, None].astype(ys.dtype)
    out = jax.ops.segment_sum(ys, row_tok, num_segments=n_tok + 1)[:n_tok]
    return out.reshape(bn, s_len, d).astype(h.dtype)


def setup_inputs(seed: int = 0) -> dict:
    key = jax.random.key(seed)
    ks = jax.random.split(key, 20)
    f32 = jnp.float32
    L, D, E, F = DEPTH, D_MODEL, N_EXPERTS, D_EXPERT
    nrm = lambda k, shape: jax.random.normal(k, shape, f32)
    x = nrm(ks[0], (BATCH, SEQ, D))
    col_scale = jnp.concatenate([jnp.full((wd,), DEEPNORM_BETA if i in VALUE_COLS else 1.0, f32) for i, wd in enumerate(PROJ_SPLITS)])
    w_in = nrm(ks[1], (L, D, PROJ_WIDTH)) * (D ** -0.5) * col_scale
    conv_w = nrm(ks[2], (L, CONV_K, 3 * DN_WIDTH)) * (CONV_K ** -0.5)
    a_log = jnp.log(jax.random.uniform(ks[3], (L, DN_HEADS), f32, 1.0, 16.0))
    dt = jnp.exp(jax.random.uniform(ks[4], (L, DN_HEADS), f32, math.log(1e-3), math.log(1e-1)))
    dt_bias = dt + jnp.log(-jnp.expm1(-dt))
    onorm_g = 1.0 + 0.02 * nrm(ks[5], (L, DN_HEAD_DIM))
    w_br_a = nrm(ks[6], (L, SB_WIDTH, D)) * (SB_WIDTH ** -0.5) * DEEPNORM_BETA
    w_br_b = nrm(ks[7], (L, DN_WIDTH, D)) * (DN_WIDTH ** -0.5) * DEEPNORM_BETA
    w_o = nrm(ks[8], (L, D, D)) * (D ** -0.5) * DEEPNORM_BETA
    ln1_g = 1.0 + 0.02 * nrm(ks[9], (L, D))
    ln1_b = 0.01 * nrm(ks[10], (L, D))
    w_router = nrm(ks[11], (D, E)) * (D ** -0.5)
    router_bias = 0.01 * nrm(ks[12], (E,))
    w_gate = nrm(ks[13], (L, E, D, F)) * (D ** -0.5) * DEEPNORM_BETA
    w_up = nrm(ks[14], (L, E, D, F)) * (D ** -0.5) * DEEPNORM_BETA
    w_down = nrm(ks[15], (L, E, F, D)) * (F ** -0.5) * DEEPNORM_BETA
    ln2_g = 1.0 + 0.02 * nrm(ks[16], (L, D))
    ln2_b = 0.01 * nrm(ks[17], (L, D))
    return {"x": x, "w_in": w_in, "conv_w": conv_w, "a_log": a_log, "dt_bias": dt_bias, "onorm_g": onorm_g, "w_br_a": w_br_a, "w_br_b": w_br_b, "w_o": w_o, "ln1_g": ln1_g, "ln1_b": ln1_b, "w_router": w_router, "router_bias": router_bias, "w_gate": w_gate, "w_up": w_up, "w_down": w_down, "ln2_g": ln2_g, "ln2_b": ln2_b}


def reference(x, w_in, conv_w, a_log, dt_bias, onorm_g, w_br_a, w_br_b, w_o, ln1_g, ln1_b, w_router, router_bias, w_gate, w_up, w_down, ln2_g, ln2_b):
    for l in range(DEPTH):
        mix = hybrid_mixer(x, w_in[l], conv_w[l], a_log[l], dt_bias[l], onorm_g[l], w_br_a[l], w_br_b[l], w_o[l])
        x = layer_norm(DEEPNORM_ALPHA * x + mix, ln1_g[l], ln1_b[l])
        ffn = grouped_moe(x, w_router, router_bias, w_gate[l], w_up[l], w_down[l])
        x = layer_norm(DEEPNORM_ALPHA * x + ffn, ln2_g[l], ln2_b[l])
    return x
```

```python
import numpy as np
import os
from contextlib import ExitStack
CSTOP = int(os.environ.get("K_CSTOP", "0"))
import concourse.bass as bass
import concourse.mybir as mybir
from concourse.bass_utils import run_bass_kernel_spmd

F32 = mybir.dt.float32
BF16 = mybir.dt.bfloat16
AF = mybir.ActivationFunctionType
ALU = mybir.AluOpType
AX = mybir.AxisListType

D = 1024
KC = 8
PW = 5640
NE = 16
FE = 512
ALPHA = 8.0 ** 0.25
NEG = -30000.0
EXPMIN = -60.0
O_QA, O_KA, O_VA, O_QB, O_KB, O_VB, O_ZB, O_BT, O_DC, O_GA, O_GB = 0, 512, 1024, 1536, 2048, 2560, 3072, 3584, 3588, 3592, 4616


class Buf:
    __slots__ = ("name", "lw", "rd")

    def __init__(self, name=""):
        self.name = name
        self.lw = None
        self.rd = {}


class Prog:
    ENGS = ("pe", "act", "dve", "pool", "sp")

    def __init__(self, nc, n_dma_sems=14):
        self.nc = nc
        self.ops = {e: [] for e in self.ENGS}
        self.cnt = {e: 0 for e in self.ENGS}
        self.known = {e: {} for e in self.ENGS}
        self.n_dma_sems = n_dma_sems
        self.dma_rr = {q: 0 for q in ("sp", "act", "pool")}
        self.dma_cnt = {}
        self.sem_keys = list(self.ENGS)
        for q in ("sp", "act", "pool"):
            for i in range(n_dma_sems):
                self.sem_keys.append(("dma", q, i))
                self.dma_cnt[("dma", q, i)] = 0

    def _collect(self, eng, reads, writes):
        waits = {}

        def add(ev):
            if ev is None:
                return
            k, v = ev[0], ev[1]
            if waits.get(k, 0) < v:
                waits[k] = v
        for b in reads:
            add(b.lw)
        for b in writes:
            add(b.lw)
            for ev in b.rd.values():
                add(ev)
        out = []
        kn = self.known[eng]
        for k, v in waits.items():
            if k == eng and eng == "pe":
                continue
            if kn.get(k, 0) >= v:
                continue
            kn[k] = v
            out.append((k, v))
        return out

    def op(self, eng, fn, reads=(), writes=()):
        waits = self._collect(eng, reads, writes)
        self.cnt[eng] += 1
        n = self.cnt[eng]
        self.ops[eng].append((waits, fn, (eng, 1)))
        ev = (eng, n)
        for b in reads:
            b.rd[eng] = ev
        for b in writes:
            b.lw = ev
            b.rd = {}
        return ev

    def dma(self, q, fn, reads=(), writes=()):
        i = self.dma_rr[q]
        self.dma_rr[q] = (i + 1) % self.n_dma_sems
        key = ("dma", q, i)
        waits = self._collect(q, reads, writes)
        prev = self.dma_cnt[key]
        if prev > 0:
            kn = self.known[q]
            if kn.get(key, 0) < 16 * prev:
                kn[key] = 16 * prev
                waits.append((key, 16 * prev))
        self.dma_cnt[key] = prev + 1
        ev = (key, 16 * (prev + 1))
        self.ops[q].append((waits, fn, (key, 16)))
        for b in reads:
            b.rd[key] = ev
        for b in writes:
            b.lw = ev
            b.rd = {}
        return ev

    def barrier(self):
        for eng in self.ENGS:
            waits = []
            kn = self.known[eng]
            for e in self.ENGS:
                if self.cnt[e] > 0 and e != eng and kn.get(e, 0) < self.cnt[e]:
                    kn[e] = self.cnt[e]
                    waits.append((e, self.cnt[e]))
            for k, c in self.dma_cnt.items():
                if c > 0 and kn.get(k, 0) < 16 * c:
                    kn[k] = 16 * c
                    waits.append((k, 16 * c))
            if waits:
                self.ops[eng].append((waits, None, None))

    def emit(self):
        nc = self.nc
        with ExitStack() as st:
            sems = {}
            for k in self.sem_keys:
                nm = k if isinstance(k, str) else "d_%s_%d" % (k[1], k[2])
                sems[k] = st.enter_context(nc.semaphore("s_" + nm))
            block = st.enter_context(nc.Block())

            def run(eng_obj, lst):
                for waits, fn, inc in lst:
                    for k, v in waits:
                        eng_obj.wait_ge(sems[k], v)
                    if fn is not None:
                        fn(eng_obj).then_inc(sems[inc[0]], inc[1])

            ops = self.ops

            @block.tensor
            def _(e):
                run(e, ops["pe"])

            @block.scalar
            def _(e):
                run(e, ops["act"])

            @block.vector
            def _(e):
                run(e, ops["dve"])

            @block.gpsimd
            def _(e):
                run(e, ops["pool"])

            @block.sync
            def _(e):
                run(e, ops["sp"])


class T:
    __slots__ = ("ap", "b")

    def __init__(self, ap, name=""):
        self.ap = ap
        self.b = Buf(name)


class Rot:
    def __init__(self, items):
        self.items = items
        self.i = 0

    def next(self):
        t = self.items[self.i]
        self.i = (self.i + 1) % len(self.items)
        return t


C_IDENT, C_ONES, C_TRIBLK, C_BLKONES, C_MINCL, C_MINCLT, C_STRICT, C_TRIN, C_NEGONES, C_MBIAS, C_END = [i * 128 for i in range(11)]


def make_consts():
    c = np.zeros((128, C_END), np.float32)
    p = np.arange(128)[:, None]
    j = np.arange(128)[None, :]
    same = (p // 64) == (j // 64)
    c[:, C_IDENT:C_IDENT + 128] = (p == j)
    c[:, C_ONES:C_ONES + 128] = 1.0
    c[:, C_TRIBLK:C_TRIBLK + 128] = same & (p <= j)
    c[:, C_BLKONES:C_BLKONES + 128] = same
    c[:, C_MINCL:C_MINCL + 128] = np.where(same & (p >= j), 0.0, NEG)
    c[:, C_MINCLT:C_MINCLT + 128] = np.where(same & (j >= p), 0.0, NEG)
    c[:, C_STRICT:C_STRICT + 128] = same & (p > j)
    c[:, C_TRIN:C_TRIN + 128] = -1.0 * (p >= j)
    c[:, C_NEGONES:C_NEGONES + 128] = -1.0
    c[:, C_MBIAS:C_MBIAS + 128] = np.where(p >= j, NEG, 0.0)
    return c


def build(S, L, dbg=(), stop_after=None):
    nc = bass.Bass("TRN2", target_bir_lowering=False)
    NT = S // 128
    NQ = S // 512
    p = Prog(nc)
    st = ExitStack()

    def din(name, shape, dt=F32):
        return nc.dram_tensor(name, list(shape), dt, kind="ExternalInput").ap()

    def dscr(name, shape, dt=F32):
        kind = "ExternalOutput" if name in dbg else "Internal"
        return T(nc.dram_tensor(name, list(shape), dt, kind=kind).ap(), name)

    x_in = din("x", [S, D])
    w_in = din("w_in", [L, D, PW])
    convw = din("convw", [L, 128, 12, 4])
    alog_b = din("alog_b", [L, 128, 4])
    dtb_b = din("dtb_b", [L, 128, 4])
    onorm_b = din("onorm_b", [L, 128, 128])
    w_br_a = din("w_br_a", [L, 512, D])
    w_br_b = din("w_br_b", [L, 512, D])
    w_o = din("w_o", [L, D, D])
    ln1g = din("ln1g", [L, 128, D])
    ln1b = din("ln1b", [L, 128, D])
    ln2g = din("ln2g", [L, 128, D])
    ln2b = din("ln2b", [L, 128, D])
    w_router = din("w_router", [D, NE])
    rbias_b = din("rbias_b", [128, NE])
    w_gate = din("w_gate", [L, NE, D, FE])
    w_up = din("w_up", [L, NE, D, FE])
    w_down = din("w_down", [L, NE, FE, D])
    consts_in = din("consts", [128, C_END])
    out_t = T(nc.dram_tensor("out", [S, D], F32, kind="ExternalOutput").ap(), "out")

    xs = [dscr("xs0", [S, D]), dscr("xs1", [S, D])]
    hs = dscr("hs", [S, D])
    qaT = dscr("qaT", [512, S], BF16)
    kaT = dscr("kaT", [512, S], BF16)
    va = dscr("va", [S, 512], BF16)
    qbT = dscr("qbT", [512, S])
    kbT = dscr("kbT", [512, S])
    vbT = dscr("vbT", [512, S])
    szb = dscr("szb", [S, 512])
    btg = dscr("btg", [S, 8])
    sgT = dscr("sgT", [2048, S])
    oaT = dscr("oaT", [512, S], BF16)
    obT = dscr("obT", [512, S], BF16)
    wts_d = dscr("wts", [S, NE])
    dbgA = dscr("dbgA", [12, 128, 512], BF16)
    dbgC = dscr("dbgC", [16, 128, 256])

    ARENA_COLS = 51200
    arena = st.enter_context(nc.sbuf_tensor("arena", [128, ARENA_COLS], F32))
    cst = st.enter_context(nc.sbuf_tensor("cst", [128, C_END], F32))
    cstb = st.enter_context(nc.sbuf_tensor("cstb", [128, C_END], BF16))
    psum = Rot([T(st.enter_context(nc.psum_tensor("ps%d" % i, [128, 512], F32))[:], "ps%d" % i) for i in range(8)])
    CST = Buf("cst")

    class Arena:
        def __init__(self):
            self.off = 0

        def reset(self):
            self.off = 0

        def f32(self, cols, name=""):
            a = arena[:, self.off:self.off + cols]
            self.off += cols
            assert self.off <= ARENA_COLS, (name, self.off)
            return T(a, name)

        def bf16(self, cols, name=""):
            n = (cols + 1) // 2
            a = arena[:, self.off:self.off + n].bitcast(BF16)
            self.off += n
            assert self.off <= ARENA_COLS, (name, self.off)
            return T(a[:, 0:cols], name)

    ar = Arena()

    def cs(off, n=128, rows=slice(0, 128)):
        return cst[rows, off:off + n]

    def csb(off, n=128, rows=slice(0, 128)):
        return cstb[rows, off:off + n]

    def ld(q, dst, src_ap, src_b=None, dst_ap=None):
        d_ap = dst.ap if dst_ap is None else dst_ap
        p.dma(q, lambda e: e.dma_start(out=d_ap, in_=src_ap), reads=[src_b] if src_b else [], writes=[dst.b])

    def stq(q, dst, dst_ap, src, src_ap=None):
        s_ap = src.ap if src_ap is None else src_ap
        p.dma(q, lambda e: e.dma_start(out=dst_ap, in_=s_ap), reads=[src.b], writes=[dst.b])

    def mm(out_ap, ob, lhsT, rhs, rd, start=True, stop=True):
        p.op("pe", lambda e: e.matmul(out_ap, lhsT=lhsT, rhs=rhs, start=start, stop=stop, skip_group_check=True), reads=rd, writes=[ob])

    def tr(out_ap, ob, in_ap, rd):
        p.op("pe", lambda e: e.transpose(out_ap, in_ap, cs(C_IDENT)), reads=rd + [CST], writes=[ob])

    def act(out_ap, in_ap, func, rd, wr, bias=None, scale=None, accum=None):
        kw = {}
        if bias is not None:
            kw["bias"] = bias
        if scale is not None:
            kw["scale"] = scale
        if accum is not None:
            kw["accum_out"] = accum
        p.op("act", lambda e: e.activation(out=out_ap, in_=in_ap, func=func, **kw), reads=rd, writes=wr)

    def tt(eng, out_ap, in0, in1, op, rd, wr):
        p.op(eng, lambda e: e.tensor_tensor(out=out_ap, in0=in0, in1=in1, op=op), reads=rd, writes=wr)

    def ts(eng, out_ap, in0, s1, op0, rd, wr, s2=None, op1=None):
        if op1 is None:
            p.op(eng, lambda e: e.tensor_scalar(out=out_ap, in0=in0, scalar1=s1, scalar2=None, op0=op0), reads=rd, writes=wr)
        else:
            p.op(eng, lambda e: e.tensor_scalar(out=out_ap, in0=in0, scalar1=s1, scalar2=s2, op0=op0, op1=op1), reads=rd, writes=wr)

    def stt(eng, out_ap, in0, scalar, in1, op0, op1, rd, wr):
        eng = "dve"
        p.op(eng, lambda e: e.scalar_tensor_tensor(out=out_ap, in0=in0, scalar=scalar, in1=in1, op0=op0, op1=op1), reads=rd, writes=wr)

    def cp(eng, out_ap, in_ap, rd, wr):
        if eng == "act":
            p.op("act", lambda e: e.copy(out_ap, in_ap), reads=rd, writes=wr)
        else:
            p.op(eng, lambda e: e.tensor_copy(out_ap, in_ap), reads=rd, writes=wr)

    def dma(q, out_ap, in_ap, rd, wr):
        p.dma(q, lambda e, o_=out_ap, i_=in_ap: e.dma_start(out=o_, in_=i_), reads=rd, writes=wr)

    def gen(eng, rd, wr, meth, *a, **k):
        p.op(eng, lambda e: getattr(e, meth)(*a, **k), reads=rd, writes=wr)

    p.dma("sp", lambda e: e.dma_start(out=cst[:], in_=consts_in), writes=[CST])
    p.op("dve", lambda e: e.tensor_copy(cstb[:], cst[:]), reads=[CST], writes=[CST])
    p.barrier()

    def layer_norm_tile(v, g_t, b_t, out_t_, small):
        stats = small.next()
        for c in range(2):
            gen("dve", [v.b], [stats.b], "bn_stats", stats.ap[:, c * 6:(c + 1) * 6], v.ap[:, c * 512:(c + 1) * 512])
        gen("dve", [stats.b], [stats.b], "bn_aggr", stats.ap[:, 12:14], stats.ap[:, 0:12])
        act(stats.ap[:, 14:15], stats.ap[:, 13:14], AF.Sqrt, [stats.b], [stats.b], bias=1e-5)
        gen("dve", [stats.b], [stats.b], "reciprocal", stats.ap[:, 15:16], stats.ap[:, 14:15])
        ts("dve", out_t_.ap, v.ap, stats.ap[:, 12:13], ALU.subtract, [v.b, stats.b], [out_t_.b], s2=stats.ap[:, 15:16], op1=ALU.mult)
        tt("pool", out_t_.ap, out_t_.ap, g_t.ap, ALU.mult, [out_t_.b, g_t.b], [out_t_.b])
        tt("pool", out_t_.ap, out_t_.ap, b_t.ap, ALU.add, [out_t_.b, b_t.b], [out_t_.b])

    def layer(l):
        x_src_ap, x_src_b = (x_in, None) if l == 0 else (xs[(l - 1) % 2].ap, xs[(l - 1) % 2].b)
        x_dst = out_t if l == L - 1 else xs[l % 2]

        p.barrier()
        ar.reset()
        win = ar.bf16(KC * PW, "win")
        winv = win.ap.rearrange("p (k n) -> p k n", k=KC)
        w_in_v = w_in[l].rearrange("(k p) n -> p k n", p=128)
        for k in range(KC):
            for c in range(4):
                c0, c1 = c * 1410, (c + 1) * 1410
                dma("pool", winv[:, k, c0:c1], w_in_v[:, k, c0:c1], [], [win.b])
        cw = ar.f32(48, "cw")
        ld("sp", cw, convw[l].rearrange("p c k -> p (c k)"))
        nealog = ar.f32(4, "nealog")
        dtb = ar.f32(4, "dtb")
        ld("sp", nealog, alog_b[l])
        ld("sp", dtb, dtb_b[l])
        act(nealog.ap, nealog.ap, AF.Exp, [nealog.b], [nealog.b])
        ts("dve", nealog.ap, nealog.ap, -1.0, ALU.mult, [nealog.b], [nealog.b])
        xt_r = Rot([ar.f32(4 * D, "xt%d" % i) for i in range(2)])
        xT = ar.bf16(KC * 512, "xT")
        xTv = xT.ap.rearrange("p (k n) -> p k n", k=KC)
        pc = [ar.f32(515, "pc%d" % i) for i in range(12)]
        for t_ in pc:
            gen("pool", [], [t_.b], "memset", t_.ap[:, 0:3], 0.0)
        stg = Rot([ar.f32(512, "stg%d" % i) for i in range(8)])
        stgb = Rot([ar.bf16(512, "stgb%d" % i) for i in range(4)])
        sm = Rot([ar.f32(16, "sm%d" % i) for i in range(4)])

        for qt in range(NQ):
            t0 = qt * 512
            xt = xt_r.next()
            xtv = xt.ap.rearrange("p (a d) -> p a d", a=4)
            for a in range(4):
                dma("sp", xtv[:, a, :], x_src_ap[t0 + a * 128:t0 + (a + 1) * 128, :], [x_src_b] if x_src_b else [], [xt.b])
            for k in range(KC):
                ps = psum.next()
                for a in range(4):
                    tr(ps.ap[:, a * 128:(a + 1) * 128], ps.b, xtv[:, a, k * 128:(k + 1) * 128], [xt.b])
                cp("act" if k % 2 else "dve", xTv[:, k, :], ps.ap, [ps.b], [xT.b])

            def fm(col0):
                ps = psum.next()
                for k in range(KC):
                    mm(ps.ap, ps.b, winv[:, k, col0:col0 + 128], xTv[:, k, :], [win.b, xT.b], start=(k == 0), stop=(k == KC - 1))
                return ps

            for c in range(16):
                ps = fm(O_GA + c * 128)
                o = stg.next()
                act(o.ap, ps.ap, AF.Sigmoid, [ps.b], [o.b])
                stq("sp", sgT, sgT.ap[c * 128:(c + 1) * 128, t0:t0 + 512], o)
            for a in range(4):
                ps = psum.next()
                for k in range(KC):
                    mm(ps.ap[:, 0:8], ps.b, xTv[:, k, a * 128:(a + 1) * 128], winv[:, k, O_BT:O_BT + 8], [win.b, xT.b], start=(k == 0), stop=(k == KC - 1))
                o = sm.next()
                tt("dve", o.ap[:, 8:12], ps.ap[:, 4:8], dtb.ap, ALU.add, [ps.b, dtb.b], [o.b])
                cp("dve", o.ap[:, 0:4], ps.ap[:, 0:4], [ps.b], [o.b])
                act(o.ap[:, 0:4], o.ap[:, 0:4], AF.Sigmoid, [o.b], [o.b])
                act(o.ap[:, 8:12], o.ap[:, 8:12], AF.Exp, [o.b], [o.b])
                act(o.ap[:, 8:12], o.ap[:, 8:12], AF.Ln, [o.b], [o.b], bias=1.0)
                tt("dve", o.ap[:, 4:8], o.ap[:, 8:12], nealog.ap, ALU.mult, [o.b, nealog.b], [o.b])
                stq("sp", btg, btg.ap[t0 + a * 128:t0 + (a + 1) * 128, :], o, o.ap[:, 0:8])
            for c in range(12):
                ps = fm(O_QB + c * 128)
                pcc = pc[c]
                cp("act", pcc.ap[:, 3:515], ps.ap, [ps.b], [pcc.b])
                acc = stg.next()
                ts("dve", acc.ap, pcc.ap[:, 3:515], cw.ap[:, c * 4 + 3:c * 4 + 4], ALU.mult, [pcc.b, cw.b], [acc.b])
                for j in (2, 1, 0):
                    stt("dve" if j == 1 else "pool", acc.ap, pcc.ap[:, j:j + 512], cw.ap[:, c * 4 + j:c * 4 + j + 1], acc.ap, ALU.mult, ALU.add, [pcc.b, cw.b, acc.b], [acc.b])
                cp("pool", pcc.ap[:, 0:3], pcc.ap[:, 512:515], [pcc.b], [pcc.b])
                act(acc.ap, acc.ap, AF.Silu, [acc.b], [acc.b])
                if c < 8:
                    sq = stg.next()
                    tt("pool", sq.ap, acc.ap, acc.ap, ALU.mult, [acc.b], [sq.b])
                    ps2 = psum.next()
                    mm(ps2.ap, ps2.b, cs(C_ONES), sq.ap, [CST, sq.b])
                    act(sq.ap, ps2.ap, AF.Sqrt, [ps2.b], [sq.b], bias=1e-6)
                    gen("dve", [sq.b], [sq.b], "reciprocal", sq.ap, sq.ap)
                    if c < 4:
                        stt("dve", acc.ap, acc.ap, 128.0 ** -0.5, sq.ap, ALU.mult, ALU.mult, [acc.b, sq.b], [acc.b])
                    else:
                        tt("dve", acc.ap, acc.ap, sq.ap, ALU.mult, [acc.b, sq.b], [acc.b])
                dstT = (qbT, kbT, vbT)[c // 4]
                stq("sp", dstT, dstT.ap[(c % 4) * 128:(c % 4 + 1) * 128, t0:t0 + 512], acc)
            for a in range(4):
                ps = psum.next()
                for k in range(KC):
                    mm(ps.ap, ps.b, xTv[:, k, a * 128:(a + 1) * 128], winv[:, k, O_ZB:O_ZB + 512], [win.b, xT.b], start=(k == 0), stop=(k == KC - 1))
                o = stg.next()
                act(o.ap, ps.ap, AF.Silu, [ps.b], [o.b])
                stq("sp", szb, szb.ap[t0 + a * 128:t0 + (a + 1) * 128, :], o)
            for c in range(8):
                ps = fm(O_QA + c * 128)
                o = stgb.next()
                if c < 4:
                    act(o.ap, ps.ap, AF.Copy, [ps.b], [o.b], scale=0.125)
                else:
                    cp("dve", o.ap, ps.ap, [ps.b], [o.b])
                dstT = qaT if c < 4 else kaT
                stq("sp", dstT, dstT.ap[(c % 4) * 128:(c % 4 + 1) * 128, t0:t0 + 512], o)
            for a in range(4):
                ps = psum.next()
                for k in range(KC):
                    mm(ps.ap, ps.b, xTv[:, k, a * 128:(a + 1) * 128], winv[:, k, O_VA:O_VA + 512], [win.b, xT.b], start=(k == 0), stop=(k == KC - 1))
                o = stgb.next()
                cp("act" if a % 2 else "dve", o.ap, ps.ap, [ps.b], [o.b])
                stq("sp", va, va.ap[t0 + a * 128:t0 + (a + 1) * 128, :], o)

        if stop_after == "A":
            return
        p.barrier()
        ar.reset()
        qk_r = Rot([(ar.bf16(S, "q2_%d" % i), ar.bf16(S, "k2_%d" % i), ar.bf16(NT * 128, "v2_%d" % i)) for i in range(2)])
        e_r = Rot([ar.f32(512, "e%d" % i) for i in range(3)])
        sp_r = Rot([ar.bf16(512, "sp%d" % i) for i in range(4)])
        a_r = Rot([ar.bf16(512, "a%d" % i) for i in range(4)])
        ss_r = Rot([tuple(ar.bf16(512, "ss%d_%d" % (i, j)) for j in range(3)) for i in range(2)])
        ost_r = Rot([ar.bf16(512, "ost%d" % i) for i in range(2)])
        zer = ar.bf16(64, "zer")
        gen("pool", [], [zer.b], "memset", zer.ap, 0.0)
        zps_r = Rot([psum.items[0], psum.items[1]])
        xps_r = Rot([psum.items[2], psum.items[3], psum.items[4]])
        ops_r = Rot([psum.items[5], psum.items[6]])

        def load_pair(hp):
            q2, k2, v2 = qk_r.next()
            CH = min(S, 2048)
            for c in range(0, S, CH):
                ld("sp", q2, qaT.ap[hp * 128:(hp + 1) * 128, c:c + CH], qaT.b, q2.ap[:, c:c + CH])
                ld("sp", k2, kaT.ap[hp * 128:(hp + 1) * 128, c:c + CH], kaT.b, k2.ap[:, c:c + CH])
            v2v = v2.ap.rearrange("p (n c) -> p n c", c=128)
            vav = va.ap.rearrange("(n p) c -> p n c", p=128)
            for n0 in range(0, NT, 8):
                n8 = min(8, NT - n0)
                dma("sp", v2v[:, n0:n0 + n8, :], vav[:, n0:n0 + n8, hp * 128:(hp + 1) * 128], [va.b], [v2.b])
            return q2, k2, v2

        nxt = load_pair(0)
        for hp in range(4):
            q2, k2, v2 = nxt
            if hp + 1 < 4:
                nxt = load_pair(hp + 1)
            v2v = v2.ap.rearrange("p (n c) -> p n c", c=128)
            steps = []
            for qt in range(NQ):
                for h in range(2):
                    trip = ss_r.next()
                    ops_ = ops_r.next()
                    kbs = list(range(4 * qt + 3, -1, -1))
                    for i, kb in enumerate(kbs):
                        j = kb - 4 * qt
                        c0 = 128 * j if j >= 0 else 0
                        steps.append(dict(qt=qt, h=h, kb=kb, c0=c0, diag=(j >= 0), first=(i == 0), last=(i == len(kbs) - 1),
                                          ss_in=trip[(i + 2) % 3], ss_out=trip[i % 3], ops=ops_, trip=trip))

            def qslice(s_):
                r = slice(s_["h"] * 64, s_["h"] * 64 + 64)
                return r, s_["qt"] * 512 + s_["c0"], s_["qt"] * 512 + 512

            def emit_Z(s_):
                r, qa, qb = qslice(s_)
                c0 = s_["c0"]
                zp = zps_r.next()
                s_["zp"] = zp
                kb = s_["kb"]
                mm(zp.ap[:, c0:512], zp.b, k2.ap[r, kb * 128:(kb + 1) * 128], q2.ap[r, qa:qb], [k2.b, q2.b], start=True, stop=not s_["diag"])
                if s_["diag"]:
                    mm(zp.ap[:, c0:c0 + 128], zp.b, csb(C_IDENT), csb(C_MBIAS), [CST], start=False, stop=True)
                e_ = e_r.next()
                act(e_.ap[:, c0:512], zp.ap[:, c0:512], AF.Exp, [zp.b], [e_.b])
                sp_ = sp_r.next()
                s_["sp"] = sp_
                act(sp_.ap[:, c0:512], e_.ap[:, c0:512], AF.Ln, [e_.b], [sp_.b], bias=1.0)
                if not s_["last"]:
                    so = s_["ss_out"]
                    if s_["first"]:
                        for j_ in range(3):
                            tb_ = s_["trip"][j_]
                            gen("pool", [], [tb_.b], "memset", tb_.ap[:, 256 - 128 * j_:384 - 128 * j_], 0.0)
                        cp("dve", so.ap[:, c0:512], sp_.ap[:, c0:512], [sp_.b], [so.b])
                    else:
                        si = s_["ss_in"]
                        tt("dve", so.ap[:, c0:512], si.ap[:, c0:512], sp_.ap[:, c0:512], ALU.add, [si.b, sp_.b], [so.b])

            def emit_X(s_):
                r, qa, qb = qslice(s_)
                c0 = s_["c0"]
                kb = s_["kb"]
                xp = xps_r.next()
                sp_ = s_["sp"]
                mm(xp.ap[:, c0:512], xp.b, k2.ap[r, kb * 128:(kb + 1) * 128], q2.ap[r, qa:qb], [k2.b, q2.b], start=True, stop=False)
                fin = s_["first"] and not s_["diag"]
                mm(xp.ap[:, c0:512], xp.b, csb(C_TRIN), sp_.ap[:, c0:512], [CST, sp_.b], start=False, stop=False)
                if not s_["first"]:
                    si = s_["ss_in"]
                    mm(xp.ap[:, c0:512], xp.b, csb(C_NEGONES), si.ap[:, c0:512], [CST, si.b], start=False, stop=not s_["diag"])
                if s_["diag"]:
                    mm(xp.ap[:, c0:c0 + 128], xp.b, csb(C_IDENT), csb(C_MBIAS), [CST], start=False, stop=True)
                a_ = a_r.next()
                s_["a"] = a_
                act(a_.ap[:, c0:512], xp.ap[:, c0:512], AF.Exp, [xp.b], [a_.b])
                if "dbgA" in dbg and hp == 0 and s_["qt"] == 0 and s_["h"] == 0:
                    stq("sp", dbgA, dbgA.ap[s_["kb"], :, :], a_)
                    stq("sp", dbgA, dbgA.ap[4 + s_["kb"], :, :], sp_)
                    if not s_["first"]:
                        stq("sp", dbgA, dbgA.ap[8 + s_["kb"], :, :], s_["ss_in"])

            def emit_O(s_):
                c0 = s_["c0"]
                kb = s_["kb"]
                h = s_["h"]
                op_ = s_["ops"]
                a_ = s_["a"]
                if s_["first"]:
                    mm(op_.ap[0:64, :], op_.b, zer.ap, q2.ap[:, 0:512], [zer.b, q2.b], start=True, stop=False)
                mm(op_.ap[0:64, c0:512], op_.b, v2v[:, kb, h * 64:(h + 1) * 64], a_.ap[:, c0:512], [v2.b, a_.b], start=False, stop=s_["last"])
                if s_["last"]:
                    o = ost_r.next()
                    cp("dve", o.ap[0:64, :], op_.ap[0:64, :], [op_.b], [o.b])
                    hh = hp * 2 + h
                    stq("sp", oaT, oaT.ap[hh * 64:(hh + 1) * 64, s_["qt"] * 512:(s_["qt"] + 1) * 512], o, o.ap[0:64, :])

            n = len(steps)
            emit_Z(steps[0])
            for i in range(n):
                if i + 1 < n:
                    emit_Z(steps[i + 1])
                emit_X(steps[i])
                if i >= 1:
                    emit_O(steps[i - 1])
            emit_O(steps[n - 1])

        if stop_after == "B":
            return
        p.barrier()
        ar.reset()
        btg_s = ar.f32(NT * 8, "btg_s")
        btgv = btg_s.ap.rearrange("p (n c) -> p n c", c=8)
        btg_dv = btg.ap.rearrange("(n p) c -> p n c", p=128)
        for n0 in range(0, NT, 16):
            n1 = min(NT, n0 + 16)
            dma("sp", btgv[:, n0:n1, :], btg_dv[:, n0:n1, :], [btg.b], [btg_s.b])
        onb = ar.f32(128, "onb")
        ld("sp", onb, onorm_b[l])
        qkv_r = Rot([tuple(ar.f32(4 * 512, "qkv%d_%d" % (i, j)) for j in range(3)) for i in range(2)])
        szb_r = Rot([ar.f32(512, "szb%d" % i) for i in range(3)])
        gsm_r = Rot([ar.f32(32, "gsm%d" % i) for i in range(3)])
        Sst = [Rot([ar.f32(128, "S%d_%d" % (h, i)) for i in range(2)]) for h in range(4)]
        Scur = [None] * 4
        for h in range(4):
            s0 = Sst[h].next()
            gen("pool", [], [s0.b], "memset", s0.ap, 0.0)
            Scur[h] = s0
        W128 = lambda nm, n: [Rot([ar.f32(128, "%s%d_%d" % (nm, h, i)) for i in range(n)]) for h in range(4)]
        gam_r, gamT_r, Gb_r, kd_r, wT_r, qG_r, PT_r, vn_r, ob_r = [W128(nm, 2) for nm in
            ("gam", "gamT", "Gb", "kd", "wT", "qG", "PT", "vn", "ob")]
        pw_r = [Rot([[(ar.bf16(128, "pw%d_%d_%d" % (h, i, lv)), ar.bf16(128, "pwT%d_%d_%d" % (h, i, lv))) for lv in range(6)] for i in range(2)]) for h in range(4)]
        X16_r = [Rot([ar.bf16(256, "X16_%d_%d" % (h, i)) for i in range(2)]) for h in range(4)]
        X_r = [Rot([ar.f32(256, "X%d_%d" % (h, i)) for i in range(2)]) for h in range(4)]
        vn2_r = [Rot([(ar.f32(128, "vnA%d_%d" % (h, i)), ar.f32(128, "vnB%d_%d" % (h, i))) for i in range(2)]) for h in range(4)]
        for h in range(4):
            for pr in vn2_r[h].items:
                for t_ in pr:
                    gen("pool", [], [t_.b], "memset", t_.ap, 0.0)
        obT_r = Rot([ar.bf16(4 * 512, "obT%d" % i) for i in range(2)])
        sm2 = Rot([ar.f32(8, "sm2_%d" % i) for i in range(8)])

        qkv = None
        for b in range(NT):
            if b % 4 == 0:
                qkv = qkv_r.next()
                for j, src in enumerate((qbT, kbT, vbT)):
                    dv = qkv[j].ap.rearrange("p (h t) -> p h t", h=4)
                    sv = src.ap.rearrange("(h p) t -> p h t", p=128)
                    dma("sp", dv, sv[:, :, b * 128:b * 128 + 512], [src.b], [qkv[j].b])
            bo = (b % 4) * 128
            qv, kv, vv = [qkv[j].ap.rearrange("p (h t) -> p h t", h=4) for j in range(3)]
            qkvb = [qkv[0].b, qkv[1].b, qkv[2].b]
            zt = szb_r.next()
            ld("sp", zt, szb.ap[b * 128:(b + 1) * 128, :], szb.b)
            gs = gsm_r.next()
            ps = psum.next()
            mm(ps.ap[:, 0:4], ps.b, cs(C_TRIBLK), btgv[:, b, 4:8], [CST, btg_s.b])
            mm(ps.ap[:, 4:8], ps.b, cs(C_BLKONES), btgv[:, b, 4:8], [CST, btg_s.b])
            cp("dve", gs.ap[:, 0:8], ps.ap[:, 0:8], [ps.b], [gs.b])
            ts("dve", gs.ap[:, 8:12], gs.ap[:, 0:4], EXPMIN, ALU.max, [gs.b], [gs.b])
            act(gs.ap[:, 8:12], gs.ap[:, 8:12], AF.Exp, [gs.b], [gs.b])
            tt("dve", gs.ap[:, 12:16], gs.ap[:, 8:12], btgv[:, b, 0:4], ALU.mult, [gs.b, btg_s.b], [gs.b])
            tt("dve", gs.ap[:, 16:20], gs.ap[:, 4:8], gs.ap[:, 0:4], ALU.subtract, [gs.b], [gs.b])
            ts("dve", gs.ap[:, 16:20], gs.ap[:, 16:20], EXPMIN, ALU.max, [gs.b], [gs.b])
            act(gs.ap[:, 16:20], gs.ap[:, 16:20], AF.Exp, [gs.b], [gs.b])
            ts("dve", gs.ap[:, 20:24], btgv[:, b, 0:4], -1.0, ALU.mult, [btg_s.b], [gs.b])
            obt = obT_r.next() if b % 4 == 0 else obt
            if CSTOP == 1:
                return
            hd = []
            for h in range(4):
                d_ = {}
                qT_ = qv[:, h, bo:bo + 128]
                kT_ = kv[:, h, bo:bo + 128]
                vT_ = vv[:, h, bo:bo + 128]
                psg = psum.next()
                mm(psg.ap[:, 0:128], psg.b, btgv[:, b, 4 + h:5 + h].to_broadcast([128, 128]), cs(C_TRIBLK), [btg_s.b, CST])
                gam, gamT, Gb = gam_r[h].next(), gamT_r[h].next(), Gb_r[h].next()
                ts("dve", gam.ap, psg.ap[:, 0:128], -1.0, ALU.mult, [psg.b, gs.b], [gam.b], s2=gs.ap[:, h:h + 1], op1=ALU.add)
                stt("dve", gam.ap, gam.ap, EXPMIN, cs(C_MINCL), ALU.max, ALU.add, [gam.b, CST], [gam.b])
                act(gam.ap, gam.ap, AF.Exp, [gam.b], [gam.b])
                ts("dve", gamT.ap, psg.ap[:, 0:128], gs.ap[:, h:h + 1], ALU.subtract, [psg.b, gs.b], [gamT.b])
                stt("dve", gamT.ap, gamT.ap, EXPMIN, cs(C_MINCLT), ALU.max, ALU.add, [gamT.b, CST], [gamT.b])
                act(gamT.ap, gamT.ap, AF.Exp, [gamT.b], [gamT.b])
                ts("pool", Gb.ap, psg.ap[:, 0:128], EXPMIN, ALU.max, [psg.b], [Gb.b]) if False else ts("dve", Gb.ap, psg.ap[:, 0:128], EXPMIN, ALU.max, [psg.b], [Gb.b])
                act(Gb.ap, Gb.ap, AF.Exp, [Gb.b], [Gb.b])
                psk = psum.next()
                mm(psk.ap[:, 0:128], psk.b, kT_, kT_, qkvb)
                mm(psk.ap[:, 128:256], psk.b, kT_, qT_, qkvb)
                tr(psk.ap[:, 256:384], psk.b, vT_, qkvb)
                tr(psk.ap[:, 384:512], psk.b, kT_, qkvb)
                pws = pw_r[h].next()
                Bm = pws[0][0]
                PT, kd, X = PT_r[h].next(), kd_r[h].next(), X_r[h].next()
                tt("dve", gam.ap, psk.ap[:, 0:128], gam.ap, ALU.mult, [psk.b, gam.b], [gam.b])
                stt("dve", Bm.ap, gam.ap, gs.ap[:, 20 + h:21 + h], cs(C_STRICT), ALU.mult, ALU.mult, [gam.b, gs.b, CST], [Bm.b])
                tt("dve", PT.ap, psk.ap[:, 128:256], gamT.ap, ALU.mult, [psk.b, gamT.b], [PT.b])
                ts("dve", X.ap[:, 0:128], psk.ap[:, 256:384], btgv[:, b, h:h + 1], ALU.mult, [psk.b, btg_s.b], [X.b])
                ts("dve", X.ap[:, 128:256], psk.ap[:, 384:512], gs.ap[:, 12 + h:13 + h], ALU.mult, [psk.b, gs.b], [X.b])
                ts("dve", kd.ap, psk.ap[:, 384:512], gs.ap[:, 16 + h:17 + h], ALU.mult, [psk.b, gs.b], [kd.b])
                qG = qG_r[h].next()
                tt("pool", qG.ap, qT_, Gb.ap, ALU.mult, [qkvb[0], Gb.b], [qG.b])
                d_.update(Bm=Bm, PT=PT, kd=kd, X=X, qG=qG, Gb=Gb, pws=pws)
                if "dbgC" in dbg and b == 0 and h == 0:
                    for i_, t_ in enumerate((gam, gamT, Gb, Bm, PT, kd, qG)):
                        stq("sp", dbgC, dbgC.ap[i_, :, 0:128], t_)
                    stq("sp", dbgC, dbgC.ap[7, :, :], X)
                    stq("sp", dbgC, dbgC.ap[8, :, 0:32], gs)
                hd.append(d_)
            if CSTOP == 2:
                return
            for h in range(4):
                d_ = hd[h]
                pst = psum.next()
                mm(pst.ap[:, 0:128], pst.b, d_["Bm"].ap, csb(C_IDENT), [d_["Bm"].b, CST])
                BT = d_["pws"][0][1]
                cp("act", BT.ap, pst.ap[:, 0:128], [pst.b], [BT.b])
                d_["pw"] = [(d_["Bm"], BT)]
                X16 = X16_r[h].next()
                d_["X16"] = X16
                cp("dve", X16.ap, d_["X"].ap, [d_["X"].b], [X16.b])
            if CSTOP == 31:
                return
            NLEV = int(os.environ.get("K_NLEV", "5"))
            SQM = int(os.environ.get("K_SQM", "0"))
            for lev in range(NLEV):
                for h in range(4):
                    Pm, PmT = hd[h]["pw"][-1]
                    ps2 = psum.next()
                    n2, n2T = hd[h]["pws"][lev + 1]
                    if SQM in (0, 1):
                        mm(ps2.ap[:, 0:128], ps2.b, PmT.ap, Pm.ap, [Pm.b, PmT.b])
                        cp("act" if h % 2 else "dve", n2.ap, ps2.ap[:, 0:128], [ps2.b], [n2.b])
                    if SQM in (0, 2):
                        mm(ps2.ap[:, 128:256], ps2.b, Pm.ap, PmT.ap, [Pm.b, PmT.b])
                        cp("act" if h % 2 else "dve", n2T.ap, ps2.ap[:, 128:256], [ps2.b], [n2T.b])
                    hd[h]["pw"].append((n2, n2T))
            if CSTOP == 32:
                return
            for lev in range(5, -1, -1):
                for h in range(4):
                    Pm, PmT = hd[h]["pw"][lev]
                    X = hd[h]["X"]
                    ps2 = psum.next()
                    X16 = hd[h]["X16"]
                    mm(ps2.ap[:, 0:256], ps2.b, PmT.ap, X16.ap, [PmT.b, X16.b])
                    tt("dve", X.ap, X.ap, ps2.ap[:, 0:256], ALU.add, [X.b, ps2.b], [X.b])
                    if lev > 0:
                        cp("act", X16.ap, X.ap, [X.b], [X16.b])
            if CSTOP == 3:
                return
            for h in range(4):
                d_ = hd[h]
                pst = psum.next()
                tr(pst.ap[:, 0:128], pst.b, d_["X"].ap[:, 128:256], [d_["X"].b])
                wT = wT_r[h].next()
                act(wT.ap, pst.ap[:, 0:128], AF.Copy, [pst.b], [wT.b], scale=-1.0)
                d_["wT"] = wT
            if CSTOP == 4:
                return
            for c in range(int(os.environ.get("K_NCH", "2"))):
                rows = slice(64 * c, 64 * c + 64)
                for h in range(4):
                    d_ = hd[h]
                    Sc = Scur[h]
                    ps1 = psum.next()
                    d_["ps1"] = ps1
                    mm(ps1.ap[:, 0:128], ps1.b, d_["wT"].ap, Sc.ap, [d_["wT"].b, Sc.b])
                    mm(ps1.ap[:, 128:256], ps1.b, d_["qG"].ap, Sc.ap, [d_["qG"].b, Sc.b], start=True, stop=False)
                SEQ = int(os.environ.get("K_SEQ", "9"))
                if SEQ < 2:
                    continue
                for h in range(4):
                    d_ = hd[h]
                    if c == 0:
                        d_["vn2"] = vn2_r[h].next()
                        d_["ob"] = ob_r[h].next()
                    vn = d_["vn2"][c]
                    d_["vn"] = vn
                    ps1 = d_["ps1"]
                    tt("dve", vn.ap[rows, :], d_["X"].ap[rows, 0:128], ps1.ap[rows, 0:128], ALU.add, [d_["X"].b, ps1.b], [vn.b])
                if SEQ < 3:
                    continue
                for h in range(4):
                    d_ = hd[h]
                    vn = d_["vn"]
                    ps1 = d_["ps1"]
                    mm(ps1.ap[:, 128:256], ps1.b, d_["PT"].ap, vn.ap, [d_["PT"].b, vn.b], start=False, stop=True)
                    mm(ps1.ap[:, 256:384], ps1.b, d_["kd"].ap, vn.ap, [d_["kd"].b, vn.b])
                if SEQ < 4:
                    continue
                for h in range(4):
                    d_ = hd[h]
                    ps1 = d_["ps1"]
                    Sc = Scur[h]
                    Sn = Sst[h].next()
                    gl = d_["Gb"].ap[:, 64 * c + 63:64 * c + 64]
                    if SEQ != 5:
                        stt("dve", Sn.ap, Sc.ap, gl, ps1.ap[:, 256:384], ALU.mult, ALU.add, [Sc.b, d_["Gb"].b, ps1.b], [Sn.b])
                        Scur[h] = Sn
                    if SEQ != 6:
                        cp("dve", d_["ob"].ap[rows, :], ps1.ap[rows, 128:256], [ps1.b], [d_["ob"].b])
            if CSTOP == 5:
                return
            pso = psum.next()
            for h in range(4):
                d_ = hd[h]
                ob = d_["ob"]
                s2 = sm2.next()
                sq = gam_r[h].next()
                act(sq.ap, ob.ap, AF.Square, [ob.b], [sq.b, s2.b], accum=s2.ap[:, 0:1])
                act(s2.ap[:, 1:2], s2.ap[:, 0:1], AF.Sqrt, [s2.b], [s2.b], scale=1.0 / 128.0, bias=1e-6)
                gen("dve", [s2.b], [s2.b], "reciprocal", s2.ap[:, 2:3], s2.ap[:, 1:2])
                stt("dve", ob.ap, ob.ap, s2.ap[:, 2:3], onb.ap, ALU.mult, ALU.mult, [ob.b, s2.b, onb.b], [ob.b])
                tt("pool", ob.ap, ob.ap, zt.ap[:, h * 128:(h + 1) * 128], ALU.mult, [ob.b, zt.b], [ob.b])
                tr(pso.ap[:, h * 128:(h + 1) * 128], pso.b, ob.ap, [ob.b])
            obtv = obt.ap.rearrange("p (h t) -> p h t", h=4)
            for h in range(4):
                cp("act", obtv[:, h, bo:bo + 128], pso.ap[:, h * 128:(h + 1) * 128], [pso.b], [obt.b])
            if b % 4 == 3:
                dv = obT.ap.rearrange("(h p) t -> p h t", p=128)
                dma("sp", dv[:, :, (b - 3) * 128:(b + 1) * 128], obtv, [obt.b], [obT.b])

        if stop_after == "C":
            return
        p.barrier()
        ar.reset()
        wbra = ar.bf16(4 * D, "wbra")
        wbrb = ar.bf16(4 * D, "wbrb")
        wo = ar.bf16(KC * D, "wo")
        wbrav = wbra.ap.rearrange("p (k n) -> p k n", k=4)
        wbrbv = wbrb.ap.rearrange("p (k n) -> p k n", k=4)
        wov = wo.ap.rearrange("p (k n) -> p k n", k=KC)
        for k in range(4):
            dma("pool", wbrav[:, k, :], w_br_a[l, k * 128:(k + 1) * 128, :], [], [wbra.b])
            dma("pool", wbrbv[:, k, :], w_br_b[l, k * 128:(k + 1) * 128, :], [], [wbrb.b])
        for k in range(KC):
            dma("pool", wov[:, k, :], w_o[l, k * 128:(k + 1) * 128, :], [], [wo.b])
        g1 = ar.f32(D, "g1")
        b1 = ar.f32(D, "b1")
        ld("sp", g1, ln1g[l])
        ld("sp", b1, ln1b[l])
        oa_r = Rot([ar.bf16(4 * 512, "oa%d" % i) for i in range(2)])
        obx_r = Rot([ar.bf16(4 * 512, "obx%d" % i) for i in range(2)])
        sg_r = Rot([ar.f32(512, "sg%d" % i) for i in range(6)])
        mT = ar.bf16(KC * 512, "mT")
        mTv = mT.ap.rearrange("p (k n) -> p k n", k=KC)
        tmp_r = Rot([ar.f32(512, "tmp%d" % i) for i in range(3)])
        xr_r = Rot([ar.f32(D, "xr%d" % i) for i in range(3)])
        sm = Rot([ar.f32(16, "smD%d" % i) for i in range(4)])
        for qt in range(NQ):
            t0 = qt * 512
            oa = oa_r.next()
            obx = obx_r.next()
            oav = oa.ap.rearrange("p (k n) -> p k n", k=4)
            obv = obx.ap.rearrange("p (k n) -> p k n", k=4)
            dma("sp", oav, oaT.ap.rearrange("(k p) t -> p k t", p=128)[:, :, t0:t0 + 512], [oaT.b], [oa.b])
            dma("sp", obv, obT.ap.rearrange("(k p) t -> p k t", p=128)[:, :, t0:t0 + 512], [obT.b], [obx.b])
            for c in range(KC):
                sga = sg_r.next()
                sgb = sg_r.next()
                ld("sp", sga, sgT.ap[c * 128:(c + 1) * 128, t0:t0 + 512], sgT.b)
                ld("sp", sgb, sgT.ap[1024 + c * 128:1024 + (c + 1) * 128, t0:t0 + 512], sgT.b)
                psa = psum.next()
                for k in range(4):
                    mm(psa.ap, psa.b, wbrav[:, k, c * 128:(c + 1) * 128], oav[:, k, :], [wbra.b, oa.b], start=(k == 0), stop=(k == 3))
                psb = psum.next()
                for k in range(4):
                    mm(psb.ap, psb.b, wbrbv[:, k, c * 128:(c + 1) * 128], obv[:, k, :], [wbrb.b, obx.b], start=(k == 0), stop=(k == 3))
                tmp = tmp_r.next()
                tt("dve", tmp.ap, psa.ap, sga.ap, ALU.mult, [psa.b, sga.b], [tmp.b])
                tt("dve", sgb.ap, psb.ap, sgb.ap, ALU.mult, [psb.b, sgb.b], [sgb.b])
                tt("pool", mTv[:, c, :], tmp.ap, sgb.ap, ALU.add, [tmp.b, sgb.b], [mT.b])
            for a in range(4):
                xr = xr_r.next()
                dma("sp", xr.ap, x_src_ap[t0 + a * 128:t0 + (a + 1) * 128, :], [x_src_b] if x_src_b else [], [xr.b])
                for g in range(2):
                    ps = psum.next()
                    for k in range(KC):
                        mm(ps.ap, ps.b, mTv[:, k, a * 128:(a + 1) * 128], wov[:, k, g * 512:(g + 1) * 512], [mT.b, wo.b], start=(k == 0), stop=(k == KC - 1))
                    stt("dve", xr.ap[:, g * 512:(g + 1) * 512], xr.ap[:, g * 512:(g + 1) * 512], ALPHA, ps.ap, ALU.mult, ALU.add, [xr.b, ps.b], [xr.b])
                layer_norm_tile(xr, g1, b1, xr, sm)
                stq("sp", hs, hs.ap[t0 + a * 128:t0 + (a + 1) * 128, :], xr)

        if stop_after == "D":
            return
        p.barrier()
        ar.reset()
        ST = min(2048, S)
        NB = ST // 128
        hT = ar.bf16(KC * ST, "hT")
        hTv = hT.ap.rearrange("p (k n) -> p k n", k=KC)
        acc = ar.f32(NB * D, "acc")
        accv = acc.ap.rearrange("p (n d) -> p n d", n=NB)
        accB = [Buf("acc%d" % n) for n in range(NB)]
        wts = ar.f32(NB * NE, "wts_s")
        wtsv = wts.ap.rearrange("p (n e) -> p n e", n=NB)
        wr = ar.f32(KC * NE, "wr")
        wrv = wr.ap.rearrange("p (k e) -> p k e", k=KC)
        ld("sp", wr, w_router.rearrange("(k p) e -> p k e", p=128), None, wrv)
        rb = ar.f32(NE, "rb")
        ld("sp", rb, rbias_b)
        g2 = ar.f32(D, "g2")
        b2 = ar.f32(D, "b2")
        ld("sp", g2, ln2g[l])
        ld("sp", b2, ln2b[l])
        wg_r = Rot([(ar.bf16(KC * FE, "wg%d" % i), ar.bf16(KC * FE, "wu%d" % i), ar.bf16(4 * D, "wd%d" % i)) for i in range(2)])
        hTf_r = Rot([ar.f32(128, "hTf%d" % i) for i in range(3)])
        sgl_r = Rot([ar.f32(512, "sgl%d" % i) for i in range(3)])
        hid_r = Rot([ar.bf16(4 * 512, "hid%d" % i) for i in range(2)])
        rt_r = Rot([ar.f32(128, "rt%d" % i) for i in range(2)])
        sm = Rot([ar.f32(16, "smE%d" % i) for i in range(4)])

        def load_expert(e_):
            wg, wu, wd = wg_r.next()
            wgv = wg.ap.rearrange("p (k n) -> p k n", k=KC)
            wuv = wu.ap.rearrange("p (k n) -> p k n", k=KC)
            wdv = wd.ap.rearrange("p (k n) -> p k n", k=4)
            for k0 in range(0, KC, 4):
                dma("pool", wgv[:, k0:k0 + 4, :], w_gate[l, e_].rearrange("(k p) n -> p k n", p=128)[:, k0:k0 + 4, :], [], [wg.b])
                dma("pool", wuv[:, k0:k0 + 4, :], w_up[l, e_].rearrange("(k p) n -> p k n", p=128)[:, k0:k0 + 4, :], [], [wu.b])
            for k0 in range(0, 4, 2):
                dma("pool", wdv[:, k0:k0 + 2, :], w_down[l, e_].rearrange("(k p) n -> p k n", p=128)[:, k0:k0 + 2, :], [], [wd.b])
            return (wg, wu, wd, wgv, wuv, wdv)

        for st0 in range(0, S, ST):
            nxtw = load_expert(0)
            for n in range(NB):
                tok = st0 + n * 128
                dma("sp", accv[:, n, :], hs.ap[tok:tok + 128, :], [hs.b], [accB[n]])
                psr = psum.next()
                for k in range(KC):
                    if k % 4 == 0:
                        ps = psum.next()
                    tr(ps.ap[:, (k % 4) * 128:(k % 4 + 1) * 128], ps.b, accv[:, n, k * 128:(k + 1) * 128], [accB[n]])
                    hTf = hTf_r.next()
                    cp("act", hTf.ap, ps.ap[:, (k % 4) * 128:(k % 4 + 1) * 128], [ps.b], [hTf.b])
                    cp("dve", hTv[:, k, n * 128:(n + 1) * 128], hTf.ap, [hTf.b], [hT.b])
                    mm(psr.ap[:, 0:NE], psr.b, hTf.ap, wrv[:, k, :], [hTf.b, wr.b], start=(k == 0), stop=(k == KC - 1))
                ESTOP = int(os.environ.get("K_ESTOP", "0"))
                if ESTOP == 1:
                    continue
                r = rt_r.next()
                R = r.ap
                s_ = sm.next()
                cp("dve", R[:, 80:96], psr.ap[:, 0:NE], [psr.b], [r.b])
                gen("dve", [r.b], [s_.b], "tensor_reduce", out=s_.ap[:, 0:1], in_=R[:, 80:96], axis=AX.X, op=ALU.max)
                ts("dve", s_.ap[:, 1:2], s_.ap[:, 0:1], -1.0, ALU.mult, [s_.b], [s_.b])
                act(R[:, 0:16], R[:, 80:96], AF.Exp, [r.b, s_.b], [r.b, s_.b], bias=s_.ap[:, 1:2], accum=s_.ap[:, 2:3])
                gen("dve", [s_.b], [s_.b], "reciprocal", s_.ap[:, 3:4], s_.ap[:, 2:3])
                ts("dve", R[:, 0:16], R[:, 0:16], s_.ap[:, 3:4], ALU.mult, [r.b, s_.b], [r.b])
                tt("dve", R[:, 16:32], R[:, 0:16], rb.ap, ALU.add, [r.b, rb.b], [r.b])
                sel3 = R[:, 16:32].rearrange("p (g e) -> p g e", g=4)
                gen("dve", [r.b], [r.b], "tensor_reduce", out=R[:, 32:36], in_=sel3, axis=AX.X, op=ALU.max)
                tt("dve", R[:, 48:64].rearrange("p (g e) -> p g e", g=4), sel3, R[:, 32:36].unsqueeze(2).to_broadcast([128, 4, 4]), ALU.is_equal, [r.b], [r.b])
                stt("dve", R[:, 48:64], R[:, 48:64], -1e9, R[:, 16:32], ALU.mult, ALU.add, [r.b], [r.b])
                gen("dve", [r.b], [r.b], "tensor_reduce", out=R[:, 36:40], in_=R[:, 48:64].rearrange("p (g e) -> p g e", g=4), axis=AX.X, op=ALU.max)
                tt("dve", R[:, 40:44], R[:, 32:36], R[:, 36:40], ALU.add, [r.b], [r.b])
                gen("dve", [r.b], [r.b], "tensor_reduce", out=R[:, 44:45], in_=R[:, 40:44], axis=AX.X, op=ALU.max)
                ts("dve", R[:, 64:68], R[:, 40:44], R[:, 44:45], ALU.is_equal, [r.b], [r.b])
                tt("dve", R[:, 48:64].rearrange("p (g e) -> p g e", g=4), sel3, R[:, 36:40].unsqueeze(2).to_broadcast([128, 4, 4]), ALU.is_ge, [r.b], [r.b])
                tt("dve", R[:, 48:64].rearrange("p (g e) -> p g e", g=4), R[:, 48:64].rearrange("p (g e) -> p g e", g=4), R[:, 64:68].unsqueeze(2).to_broadcast([128, 4, 4]), ALU.mult, [r.b], [r.b])
                tt("dve", R[:, 48:64], R[:, 48:64], R[:, 0:16], ALU.mult, [r.b], [r.b])
                gen("dve", [r.b], [r.b], "tensor_reduce", out=R[:, 45:46], in_=R[:, 48:64], axis=AX.X, op=ALU.add)
                gen("dve", [r.b], [r.b], "reciprocal", R[:, 46:47], R[:, 45:46])
                ts("dve", wtsv[:, n, :], R[:, 48:64], R[:, 46:47], ALU.mult, [r.b], [wts.b])
                if "wts" in dbg:
                    stq("sp", wts_d, wts_d.ap[tok:tok + 128, :], wts, wtsv[:, n, :])
                ts("pool", accv[:, n, :], accv[:, n, :], ALPHA, ALU.mult, [accB[n]], [accB[n]])
            if ESTOP == 1 or ESTOP == 2:
                return
            for e_ in range(NE if ESTOP == 0 else 1):
                wg, wu, wd, wgv, wuv, wdv = nxtw
                if e_ + 1 < NE:
                    nxtw = load_expert(e_ + 1)
                for tq in range(ST // 512):
                    hid = hid_r.next()
                    hidv = hid.ap.rearrange("p (f n) -> p f n", f=4)
                    for f in range(4):
                        psg_ = psum.next()
                        for k in range(KC):
                            mm(psg_.ap, psg_.b, wgv[:, k, f * 128:(f + 1) * 128], hTv[:, k, tq * 512:(tq + 1) * 512], [wg.b, hT.b], start=(k == 0), stop=(k == KC - 1))
                        psu_ = psum.next()
                        for k in range(KC):
                            mm(psu_.ap, psu_.b, wuv[:, k, f * 128:(f + 1) * 128], hTv[:, k, tq * 512:(tq + 1) * 512], [wu.b, hT.b], start=(k == 0), stop=(k == KC - 1))
                        sgl = sgl_r.next()
                        act(sgl.ap, psg_.ap, AF.Silu, [psg_.b], [sgl.b])
                        tt("dve", hidv[:, f, :], sgl.ap, psu_.ap, ALU.mult, [sgl.b, psu_.b], [hid.b])
                    for a in range(4):
                        n = tq * 4 + a
                        for g in range(2):
                            psd = psum.next()
                            for f in range(4):
                                mm(psd.ap, psd.b, hidv[:, f, a * 128:(a + 1) * 128], wdv[:, f, g * 512:(g + 1) * 512], [hid.b, wd.b], start=(f == 0), stop=(f == 3))
                            stt("dve", accv[:, n, g * 512:(g + 1) * 512], psd.ap, wtsv[:, n, e_:e_ + 1], accv[:, n, g * 512:(g + 1) * 512],
                                ALU.mult, ALU.add, [psd.b, wts.b, accB[n]], [accB[n]])
            if ESTOP == 3:
                return
            for n in range(NB):
                tok = st0 + n * 128
                vt = T(accv[:, n, :], "accn")
                vt.b = accB[n]
                layer_norm_tile(vt, g2, b2, vt, sm)
                stq("sp", x_dst, x_dst.ap[tok:tok + 128, :], vt)

    for l in range(L):
        layer(l)
    p.barrier()
    p.emit()
    st.close()
    return nc


def prep_shared(inp, L):
    f = lambda a: np.ascontiguousarray(np.asarray(a, dtype=np.float32))
    bc = lambda a: np.ascontiguousarray(np.broadcast_to(np.asarray(a, np.float32)[:, None, :], (a.shape[0], 128, a.shape[1])))
    conv_w = np.asarray(inp["conv_w"], np.float32)
    convw = np.ascontiguousarray(conv_w.reshape(-1, 4, 12, 128).transpose(0, 3, 2, 1))
    m = {
        "w_in": f(inp["w_in"]), "convw": convw,
        "alog_b": bc(inp["a_log"]), "dtb_b": bc(inp["dt_bias"]), "onorm_b": bc(inp["onorm_g"]),
        "w_br_a": f(inp["w_br_a"]), "w_br_b": f(inp["w_br_b"]), "w_o": f(inp["w_o"]),
        "ln1g": bc(inp["ln1_g"]), "ln1b": bc(inp["ln1_b"]), "ln2g": bc(inp["ln2_g"]), "ln2b": bc(inp["ln2_b"]),
        "w_router": f(inp["w_router"]),
        "rbias_b": np.ascontiguousarray(np.broadcast_to(np.asarray(inp["router_bias"], np.float32)[None, :], (128, NE))),
        "w_gate": f(inp["w_gate"]), "w_up": f(inp["w_up"]), "w_down": f(inp["w_down"]),
        "consts": make_consts(),
    }
    return m


_CACHE = {}
LAYERS_PER_LAUNCH = 1


def kernel(**inputs):
    x = np.asarray(inputs["x"], np.float32)
    B, S, _ = x.shape
    L = inputs["w_in"].shape[0]
    G = LAYERS_PER_LAUNCH
    key = (S, G)
    if key not in _CACHE:
        _CACHE[key] = build(S, G)
    nc = _CACHE[key]
    per_layer = ("w_in", "conv_w", "a_log", "dt_bias", "onorm_g", "w_br_a", "w_br_b", "w_o", "ln1_g", "ln1_b",
                 "w_gate", "w_up", "w_down", "ln2_g", "ln2_b")
    cur = [np.ascontiguousarray(x[b]) for b in range(B)]
    for l0 in range(0, L, G):
        sub = dict(inputs)
        for k in per_layer:
            sub[k] = np.asarray(inputs[k])[l0:l0 + G]
        shared = prep_shared(sub, G)
        in_maps = []
        for c in range(8):
            m = dict(shared)
            m["x"] = cur[c % B]
            in_maps.append(m)
        res = run_bass_kernel_spmd(nc, in_maps, core_ids=list(range(8)))
        cur = [np.ascontiguousarray(res.results[b]["out"]) for b in range(B)]
    return np.stack(cur, axis=0).astype(np.float32)
```
